# Optimizing a Trainium2 kernel written in Bass

```python
import jax
import jax.numpy as jnp
from jax import lax
import numpy as np

D_MODEL = 4096
BATCH = 4
SEQ = 2048
DEPTH = 1

MLA_HEADS = 16
MLA_NOPE_DIM = 128
MLA_ROPE_DIM = 64
MLA_V_DIM = 128
MLA_Q_RANK = 768
MLA_KV_RANK = 512
SWA_HEADS = 16
SWA_KV_HEADS = 4
SWA_HEAD_DIM = 128
SWA_GROUP = SWA_HEADS // SWA_KV_HEADS
WINDOW = 128
BLOCK = 128
ROPE_THETA = 10000.0
N_GROUPS = 8
EXPERTS_PER_GROUP = 8
TOP_K_IN_GROUP = 2
EXPERT_FF = 1024
PLE_DIM = 256
LN_EPS = 1e-5
RMS_EPS = 1e-6
DEEPNORM_ALPHA = (2.0 * DEPTH) ** 0.25
DEEPNORM_BETA = (8.0 * DEPTH) ** -0.25

IN_SIZES = (MLA_Q_RANK, MLA_KV_RANK, MLA_ROPE_DIM,
            SWA_HEADS * SWA_HEAD_DIM, SWA_KV_HEADS * SWA_HEAD_DIM, SWA_KV_HEADS * SWA_HEAD_DIM,
            D_MODEL, D_MODEL)
IN_WIDTH = sum(IN_SIZES)
IN_OFFSETS = tuple(int(v) for v in np.cumsum(IN_SIZES)[:-1])

kernel_name = 'hybrid_mla_swa_hmoe_encoder'


def layer_norm(x, w, b):
    xf = x.astype(jnp.float32)
    mu = jnp.mean(xf, axis=-1, keepdims=True)
    var = jnp.mean(jnp.square(xf - mu), axis=-1, keepdims=True)
    y = (xf - mu) * lax.rsqrt(var + LN_EPS) * w.astype(jnp.float32) + b.astype(jnp.float32)
    return y.astype(x.dtype)


def rms_norm(x, w):
    xf = x.astype(jnp.float32)
    y = xf * lax.rsqrt(jnp.mean(jnp.square(xf), axis=-1, keepdims=True) + RMS_EPS) * w.astype(jnp.float32)
    return y.astype(x.dtype)


def rope_tables(positions, dim):
    inv_freq = ROPE_THETA ** (-jnp.arange(0, dim, 2, dtype=jnp.float32) / dim)
    ang = positions.astype(jnp.float32)[..., None] * inv_freq
    return jnp.cos(ang), jnp.sin(ang)


def apply_rope(x, cos, sin):
    extra = x.ndim - cos.ndim
    c = cos.reshape(cos.shape[:2] + (1,) * extra + cos.shape[2:])
    s = sin.reshape(sin.shape[:2] + (1,) * extra + sin.shape[2:])
    x1, x2 = jnp.split(x.astype(jnp.float32), 2, axis=-1)
    return jnp.concatenate([x1 * c - x2 * s, x2 * c + x1 * s], axis=-1).astype(x.dtype)


def mla_attention(q_nope, q_rope, k_nope, k_rope, v):
    b, s, h, _ = q_nope.shape
    nb = s // BLOCK
    scale = (MLA_NOPE_DIM + MLA_ROPE_DIM) ** -0.5

    def to_blocks(t):
        return t.reshape((b, nb, BLOCK) + t.shape[2:]).swapaxes(0, 1)

    def one_block(qs):
        qn, qr = qs
        sc = jnp.einsum('bqhd,bkhd->bhqk', qn, k_nope, preferred_element_type=jnp.float32)
        sc = sc + jnp.einsum('bqhr,bkr->bhqk', qr, k_rope, preferred_element_type=jnp.float32)
        prob = jax.nn.softmax(sc * scale, axis=-1)
        return jnp.einsum('bhqk,bkhd->bqhd', prob.astype(v.dtype), v)

    o = lax.map(one_block, (to_blocks(q_nope), to_blocks(q_rope)))
    return o.swapaxes(0, 1).reshape(b, s, h * v.shape[-1])


def windowed_gqa_sink(q, k, v, sink):
    b, s = q.shape[:2]
    nb = s // BLOCK
    hd = q.shape[-1]
    qb = q.reshape(b, nb, BLOCK, SWA_KV_HEADS, SWA_GROUP, hd)

    def neighbourhood(t):
        tp = jnp.pad(t, ((0, 0), (BLOCK, BLOCK), (0, 0), (0, 0))).reshape(b, nb + 2, BLOCK, SWA_KV_HEADS, hd)
        return jnp.concatenate([tp[:, :-2], tp[:, 1:-1], tp[:, 2:]], axis=2)

    kw = neighbourhood(k)
    vw = neighbourhood(v)
    sc = jnp.einsum('bnqkgd,bnpkd->bnkgqp', qb, kw, preferred_element_type=jnp.float32) * (hd ** -0.5)
    blk = jnp.arange(nb)[:, None] * BLOCK
    q_pos = blk + jnp.arange(BLOCK)[None, :]
    k_pos = blk - BLOCK + jnp.arange(3 * BLOCK)[None, :]
    valid = ((jnp.abs(q_pos[:, :, None] - k_pos[:, None, :]) <= WINDOW)
             & (k_pos >= 0)[:, None, :] & (k_pos < s)[:, None, :])
    sc = jnp.where(valid[None, :, None, None], sc, -jnp.inf)
    sink_l = sink.astype(jnp.float32).reshape(1, 1, SWA_KV_HEADS, SWA_GROUP, 1, 1)
    m = jnp.maximum(jnp.max(sc, axis=-1, keepdims=True), sink_l)
    e = jnp.exp(sc - m)
    prob = e / (jnp.sum(e, axis=-1, keepdims=True) + jnp.exp(sink_l - m))
    o = jnp.einsum('bnkgqp,bnpkd->bnqkgd', prob.astype(v.dtype), vw)
    return o.reshape(b, s, SWA_HEADS * hd)


def hierarchical_moe(x, w_group, b_group, w_router, b_expert, w_gate, w_up, w_down):
    b, s, d = x.shape
    xt = x.reshape(b * s, d)
    t = xt.shape[0]
    group_logits = jnp.einsum('td,dg->tg', xt, w_group, preferred_element_type=jnp.float32) + b_group.astype(jnp.float32)
    group_prob = jax.nn.softmax(group_logits, axis=-1)
    g_w, g_idx = lax.top_k(group_prob, 1)
    g_onehot = jax.nn.one_hot(g_idx[:, 0], N_GROUPS, dtype=jnp.float32)
    expert_logits = (jnp.einsum('td,de->te', xt, w_router, preferred_element_type=jnp.float32)
                     + b_expert.astype(jnp.float32)).reshape(t, N_GROUPS, EXPERTS_PER_GROUP)
    sel_logits = jnp.sum(g_onehot[:, :, None] * expert_logits, axis=1)
    top_l, top_i = lax.top_k(sel_logits, TOP_K_IN_GROUP)
    top_w = jax.nn.softmax(top_l, axis=-1) * g_w
    within = jnp.sum(jax.nn.one_hot(top_i, EXPERTS_PER_GROUP, dtype=jnp.float32) * top_w[..., None], axis=1)
    combine = (g_onehot[:, :, None] * within[:, None, :]).astype(x.dtype)
    out = jnp.zeros_like(xt)
    for g in range(N_GROUPS):
        hg = jnp.einsum('td,edf->tef', xt, w_gate[g])
        hu = jnp.einsum('td,edf->tef', xt, w_up[g])
        act = jax.nn.silu(hg) * hu * combine[:, g, :, None]
        out = out + jnp.einsum('tef,efd->td', act, w_down[g])
    return out.reshape(b, s, d)


def setup_inputs(seed: int = 0) -> dict:
    key = jax.random.key(seed)
    ks = jax.random.split(key, 32)
    f32 = jnp.float32
    L, D = DEPTH, D_MODEL

    def dense(k, shape, fan_in, scale=1.0):
        return jax.random.normal(k, shape, f32) * (scale * fan_in ** -0.5)

    def gain(k, shape):
        return 1.0 + 0.02 * jax.random.normal(k, shape, f32)

    def small(k, shape, s):
        return s * jax.random.normal(k, shape, f32)

    return {
        'x': jax.random.normal(ks[0], (BATCH, SEQ, D), f32),
        'p': jax.random.normal(ks[1], (L, BATCH, SEQ, PLE_DIM), f32),
        'positions': jnp.tile(jnp.arange(SEQ, dtype=jnp.int32)[None, :], (BATCH, 1)),
        'w_in': dense(ks[2], (L, D, IN_WIDTH), D),
        'q_norm': gain(ks[3], (L, MLA_Q_RANK)),
        'kv_norm': gain(ks[4], (L, MLA_KV_RANK)),
        'w_q_up': dense(ks[5], (L, MLA_Q_RANK, MLA_HEADS * (MLA_NOPE_DIM + MLA_ROPE_DIM)), MLA_Q_RANK),
        'w_kv_up': dense(ks[6], (L, MLA_KV_RANK, MLA_HEADS * (MLA_NOPE_DIM + MLA_V_DIM)), MLA_KV_RANK),
        'sink': small(ks[7], (L, SWA_HEADS), 1.0),
        'w_branch_a': dense(ks[8], (L, MLA_HEADS * MLA_V_DIM, D), MLA_HEADS * MLA_V_DIM),
        'w_branch_b': dense(ks[9], (L, SWA_HEADS * SWA_HEAD_DIM, D), SWA_HEADS * SWA_HEAD_DIM),
        'w_out': dense(ks[10], (L, D, D), D, DEEPNORM_BETA),
        'ln1_w': gain(ks[11], (L, D)),
        'ln1_b': small(ks[12], (L, D), 0.02),
        'w_group': dense(ks[13], (L, D, N_GROUPS), D),
        'b_group': small(ks[14], (L, N_GROUPS), 0.01),
        'w_expert_router': dense(ks[15], (L, D, N_GROUPS * EXPERTS_PER_GROUP), D),
        'b_expert': small(ks[16], (L, N_GROUPS * EXPERTS_PER_GROUP), 0.01),
        'w_gate': dense(ks[17], (L, N_GROUPS, EXPERTS_PER_GROUP, D, EXPERT_FF), D),
        'w_up': dense(ks[18], (L, N_GROUPS, EXPERTS_PER_GROUP, D, EXPERT_FF), D),
        'w_down': dense(ks[19], (L, N_GROUPS, EXPERTS_PER_GROUP, EXPERT_FF, D), EXPERT_FF, DEEPNORM_BETA),
        'w_ple_up': dense(ks[20], (L, PLE_DIM, D), PLE_DIM, DEEPNORM_BETA),
        'w_ple_gate': dense(ks[21], (L, D, D), D),
        'ln2_w': gain(ks[22], (L, D)),
        'ln2_b': small(ks[23], (L, D), 0.02),
    }


def reference(x, p, positions, w_in, q_norm, kv_norm, w_q_up, w_kv_up, sink, w_branch_a, w_branch_b,
              w_out, ln1_w, ln1_b, w_group, b_group, w_expert_router, b_expert, w_gate, w_up, w_down,
              w_ple_up, w_ple_gate, ln2_w, ln2_b):
    b, s, _ = x.shape
    cos_r, sin_r = rope_tables(positions, MLA_ROPE_DIM)
    cos_f, sin_f = rope_tables(positions, SWA_HEAD_DIM)
    for i in range(DEPTH):
        proj = jnp.einsum('bsd,de->bse', x, w_in[i])
        c_q, c_kv, k_r, q_b, k_b, v_b, g_a, g_b = jnp.split(proj, IN_OFFSETS, axis=-1)
        q_a = (rms_norm(c_q, q_norm[i]) @ w_q_up[i]).reshape(b, s, MLA_HEADS, MLA_NOPE_DIM + MLA_ROPE_DIM)
        q_nope, q_rope = q_a[..., :MLA_NOPE_DIM], apply_rope(q_a[..., MLA_NOPE_DIM:], cos_r, sin_r)
        kv_a = (rms_norm(c_kv, kv_norm[i]) @ w_kv_up[i]).reshape(b, s, MLA_HEADS, MLA_NOPE_DIM + MLA_V_DIM)
        k_nope, v_a = kv_a[..., :MLA_NOPE_DIM], kv_a[..., MLA_NOPE_DIM:]
        k_rope = apply_rope(k_r, cos_r, sin_r)
        o_a = mla_attention(q_nope, q_rope, k_nope, k_rope, v_a)
        qs = apply_rope(q_b.reshape(b, s, SWA_HEADS, SWA_HEAD_DIM), cos_f, sin_f)
        kswa = apply_rope(k_b.reshape(b, s, SWA_KV_HEADS, SWA_HEAD_DIM), cos_f, sin_f)
        vswa = v_b.reshape(b, s, SWA_KV_HEADS, SWA_HEAD_DIM)
        o_b = windowed_gqa_sink(qs, kswa, vswa, sink[i])
        merged = jax.nn.sigmoid(g_a) * (o_a @ w_branch_a[i]) + jax.nn.sigmoid(g_b) * (o_b @ w_branch_b[i])
        mixed = merged @ w_out[i]
        x1 = layer_norm(DEEPNORM_ALPHA * x + mixed, ln1_w[i], ln1_b[i])
        moe = hierarchical_moe(x1, w_group[i], b_group[i], w_expert_router[i], b_expert[i],
                               w_gate[i], w_up[i], w_down[i])
        ple = jax.nn.sigmoid(x1 @ w_ple_gate[i]) * (p[i].astype(x1.dtype) @ w_ple_up[i])
        x = layer_norm(DEEPNORM_ALPHA * x1 + moe + ple, ln2_w[i], ln2_b[i])
    return x
```

```python
import numpy as np
import ml_dtypes
import concourse.bass as bass
import concourse.mybir as mybir
from concourse.bass_utils import run_bass_kernel_spmd

F32, BF16, I32 = mybir.dt.float32, mybir.dt.bfloat16, mybir.dt.int32
AF = mybir.ActivationFunctionType
ALU = mybir.AluOpType
AX = mybir.AxisListType
NCORES = 8


class Cfg:
    def __init__(self, **kw):
        self.D = 4096; self.S = 2048; self.B = 4
        self.H = 16; self.NOPE = 128; self.ROPE = 64; self.VD = 128; self.QR = 768; self.KR = 512
        self.HS = 16; self.HKV = 4; self.HD = 128
        self.G = 8; self.E = 8; self.FF = 1024; self.PLE = 256
        self.CH = 256; self.TB = 512; self.ARENA_KB = 142; self.KOFF_KB = 82
        self.STOP = None
        self.MOE = "sparse"
        self.CAP = 512
        self.ln_eps = 1e-5; self.rms_eps = 1e-6; self.theta = 10000.0; self.depth = 1
        for k, v in kw.items():
            setattr(self, k, v)
        self.NT = self.S // 2
        self.alpha = (2.0 * self.depth) ** 0.25
        self.GRP = self.HS // self.HKV
        self.sizes = (self.QR, self.KR, self.ROPE, self.HS * self.HD, self.HKV * self.HD, self.HKV * self.HD, self.D, self.D)
        self.offs = [0] + [int(v) for v in np.cumsum(self.sizes)[:-1]]
        self.INW = int(sum(self.sizes))


class Region:
    __slots__ = ("w", "r")

    def __init__(self):
        self.w = []
        self.r = []


def _add_tok(lst, tok):
    for i, (s, v) in enumerate(lst):
        if s is tok[0]:
            if v < tok[1]:
                lst[i] = tok
            return
    lst.append(tok)


class Buf:
    def __init__(self, t, regions=None):
        self.t = t
        self.regions = regions if regions is not None else [Region()]
        self.dsem = {}
        self.dcnt = {}

    def __getitem__(self, k):
        return self.t[k]


class Eng:
    def __init__(self, nc, e, name):
        self.e = e
        self.name = name
        self.sem = nc.alloc_semaphore("s_" + name)
        self.cnt = 0
        self.seen = {}

    def wait(self, tok):
        sem, val = tok
        k = id(sem)
        if self.seen.get(k, 0) >= val:
            return
        self.e.wait_ge(sem, val)
        self.seen[k] = val

    def mark(self, ins):
        ins.then_inc(self.sem, 1)
        self.cnt += 1
        return (self.sem, self.cnt)


class Ctx:
    def __init__(self, nc):
        self.nc = nc
        self.engs = {n: Eng(nc, e, n) for n, e in (("pe", nc.tensor), ("act", nc.scalar), ("dve", nc.vector),
                                                    ("pool", nc.gpsimd), ("sp", nc.sync))}
        self.nsem = 5
        self.dma_bufs = []

    def _waits(self, E, reads, writes, pwrites):
        for b in reads:
            for R in b.regions:
                for t in R.w:
                    E.wait(t)
        for b in writes:
            for R in b.regions:
                for t in R.w:
                    E.wait(t)
                for t in R.r:
                    E.wait(t)
        for b in pwrites:
            for R in b.regions:
                for t in R.w:
                    E.wait(t)
                for t in R.r:
                    E.wait(t)

    def _commit(self, tok, reads, writes, pwrites):
        for b in reads:
            for R in b.regions:
                _add_tok(R.r, tok)
        for b in writes:
            for R in b.regions:
                R.w = [tok]
                R.r = []
        for b in pwrites:
            for R in b.regions:
                _add_tok(R.w, tok)

    def op(self, eng, fn, reads=(), writes=(), pwrites=()):
        E = self.engs[eng]
        self._waits(E, reads, writes, pwrites)
        ins = fn(E.e)
        tok = E.mark(ins)
        self._commit(tok, reads, writes, pwrites)
        return tok

    def mm(self, out_ap, steps, reads, pbuf, pw=False, tr=False):
        E = self.engs["pe"]
        self._waits(E, reads, () if pw else (pbuf,), (pbuf,) if pw else ())
        n = len(steps)
        ins = None
        for i, (a, b) in enumerate(steps):
            ins = E.e.matmul(out_ap, a, b, start=(i == 0), stop=(i == n - 1))
        tok = E.mark(ins)
        self._commit(tok, reads, () if pw else (pbuf,), (pbuf,) if pw else ())
        return tok

    def transpose(self, out_ap, in_ap, ident_ap, reads, pbuf, pw=True):
        E = self.engs["pe"]
        self._waits(E, reads, () if pw else (pbuf,), (pbuf,) if pw else ())
        ins = E.e.transpose(out_ap, in_ap, ident_ap)
        tok = E.mark(ins)
        self._commit(tok, reads, () if pw else (pbuf,), (pbuf,) if pw else ())
        return tok

    def dma(self, q, out, in_, reads=(), writes=(), pwrites=(), owner=None, fn=None):
        E = self.engs[q]
        self._waits(E, reads, writes, pwrites)
        if owner is None:
            owner = (list(writes) + list(pwrites) + list(reads))[0]
        if q not in owner.dsem:
            owner.dsem[q] = self.nc.alloc_semaphore(f"d{self.nsem}")
            owner.dcnt[q] = 0
            self.nsem += 1
            self.dma_bufs.append((owner, q, 16))
        if fn is None:
            ins = E.e.dma_start(out=out, in_=in_)
        else:
            ins = fn(E.e)
        ins.then_inc(owner.dsem[q], 16)
        owner.dcnt[q] += 1
        tok = (owner.dsem[q], 16 * owner.dcnt[q])
        self._commit(tok, reads, writes, pwrites)
        return tok

    def finish(self):
        E = self.engs["sp"]
        for n, e2 in self.engs.items():
            if n != "sp" and e2.cnt > 0:
                E.wait((e2.sem, e2.cnt))
        for b, q, mult in self.dma_bufs:
            E.wait((b.dsem[q], mult * b.dcnt[q]))

    def wait_all(self, eng, bufs):
        E = self.engs[eng]
        for b in bufs:
            for R in b.regions:
                for t in R.w:
                    E.wait(t)
                for t in R.r:
                    E.wait(t)


def build(cfg):
    c = cfg
    P = 128
    D, NT, CH = c.D, c.NT, c.CH
    DK = D // P
    NT2 = 2 * NT
    NB = NT // P
    NQT = CH // P
    NPASS = NT // CH
    QRC, KRC = c.QR // P, c.KR // P
    H, HS, HKV, GRP = c.H, c.HS, c.HKV, c.GRP
    HVC = H * c.VD // P
    HSC = HS * c.HD // P
    QW = c.NOPE + c.ROPE
    KW = c.NOPE + c.VD
    NR = c.G + c.G * c.E
    o_cq, o_ckv, o_kr, o_qb, o_kb, o_vb, o_ga, o_gb = c.offs
    PK = c.PLE // P
    FC = c.FF // P
    TB = c.TB
    NTOT = NCORES * NT
    NTB = NTOT // TB
    assert c.ROPE == 64 and c.HD == 128 and c.NOPE == 128 and c.VD == 128
    sc_mla = float(QW) ** -0.5
    sc_swa = float(c.HD) ** -0.5

    nc = bass.Bass("TRN2", target_bir_lowering=False)
    cx = Ctx(nc)

    def din(name, shape, dt=F32):
        return Buf(nc.dram_tensor(name, list(shape), dt, kind="ExternalInput").ap())

    def dscr(name, shape, dt):
        return Buf(nc.dram_tensor(name, list(shape), dt).ap())

    x_own = din("x_own", [NT, D]); x_oth = din("x_oth", [NT, D])
    pos = din("pos", [1, NT2], I32)
    p_own = din("p_own", [NT, c.PLE])
    w_in = din("w_in", [D, c.INW]); q_norm = din("q_norm", [128, c.QR // 128]); kv_norm = din("kv_norm", [128, c.KR // 128])
    w_q_up = din("w_q_up", [c.QR, H * QW]); w_kv_up = din("w_kv_up", [c.KR, H * KW])
    sink = din("sink", [1, HS])
    w_a = din("w_a", [H * c.VD, D]); w_b = din("w_b", [HS * c.HD, D]); w_out = din("w_out", [D, D])
    ln1_w = din("ln1_w", [1, D]); ln1_b = din("ln1_b", [1, D]); ln2_w = din("ln2_w", [1, D]); ln2_b = din("ln2_b", [1, D])
    w_rt = din("w_rt", [D, NR]); b_rt = din("b_rt", [1, NR])
    w_gate = din("w_gate", [c.E, D, c.FF]); w_up = din("w_up", [c.E, D, c.FF]); w_down = din("w_down", [c.E, c.FF, D])
    w_pu = din("w_pu", [c.PLE, D]); w_pg = din("w_pg", [D, D])
    c_bf = din("c_bf", [P, 128 + 128 + 64 + 128 + 4 * 512 + 128], BF16)
    c_f = din("c_f", [P, 8])
    g_sel = din("g_sel", [1, c.G])
    e_cap = din("e_cap", [1, c.E])
    out_d = Buf(nc.dram_tensor("out", [NT, D], F32, kind="ExternalOutput").ap())

    x1_own_d = dscr("x1_own_d", [NT, D], BF16)
    x1_all_d = dscr("x1_all_d", [NTOT, D], BF16)
    base_d = dscr("base_d", [NT, D], F32)
    cmb_own_d = dscr("cmb_own_d", [NT, c.G * c.E], F32)
    cmb_all_d = dscr("cmb_all_d", [NTOT, c.G * c.E], F32)
    z_d = dscr("z_d", [NTOT, D], F32)
    moe_own_d = dscr("moe_own_d", [NT, D], F32)

    def sb(name, shape, dt):
        return Buf(nc.alloc_sbuf_tensor(name, list(shape), dt))

    ARENA_KB = c.ARENA_KB
    arena = nc.alloc_sbuf_tensor("arena", [P, ARENA_KB * 256], F32)
    gran = [Region() for _ in range(ARENA_KB // 2 + 1)]

    class ABuf(Buf):
        pass

    def A(off_kb, shape, dt):
        esz = 4 if dt in (F32, I32) else 2
        n = 1
        for v in shape[1:]:
            n *= v
        nbytes = n * esz
        assert off_kb % 2 == 0
        assert off_kb * 1024 + nbytes <= ARENA_KB * 1024, (off_kb, nbytes)
        w0 = off_kb * 256
        ap = arena[0:shape[0], w0:w0 + (nbytes + 3) // 4]
        if dt != F32:
            ap = ap.bitcast(dt)
        ap = ap[:, 0:n]
        if len(shape) == 3:
            ap = ap.rearrange("p (a b) -> p a b", b=shape[2])
        elif len(shape) == 4:
            ap = ap.rearrange("p (a b c) -> p a b c", b=shape[2], c=shape[3])
        g0 = off_kb // 2
        g1 = (off_kb * 1024 + nbytes + 2047) // 2048
        return Buf(ap, regions=gran[g0:g1])

    def ps(name, shape, dt=F32):
        return Buf(nc.alloc_psum_tensor(name, list(shape), dt))

    cbf = sb("cbf", [P, 128 + 128 + 64 + 128 + 4 * 512 + 128], BF16)
    cf = sb("cf", [P, 8], F32)
    cx.dma("sp", cbf[:], c_bf[:], reads=[c_bf], writes=[cbf])
    cx.dma("sp", cf[:], c_f[:], reads=[c_f], writes=[cf])
    ident = cbf.t[:, 0:128]
    ones_bf = cbf.t[:, 128:256]
    swap64 = cbf.t[0:64, 256:320]
    swap128 = cbf.t[:, 320:448]
    masks = cbf.t[:, 448:448 + 2048]
    utri = cbf.t[:, 2496:2624]
    CONST = [cbf, cf]

    PA = ps("PA", [P, 512]); PB = ps("PB", [P, 512]); PS0 = ps("PS0", [P, 512]); PS1 = ps("PS1", [P, 512])
    PO = ps("PO", [P, 1024]); PT = ps("PT", [P, 1024], BF16); PM = ps("PM", [P, 512])
    rot2 = [0]

    def bankAB():
        rot2[0] ^= 1
        return PA if rot2[0] else PB
    rotS = [0]

    def bankS():
        rotS[0] ^= 1
        return PS0 if rotS[0] else PS1

    NW = 3
    wslots = [sb(f"w{i}", [P, DK, 128], BF16) for i in range(NW)]
    wrot = [0]

    def wload(src_buf, rows, c0, ncols):
        s = wslots[wrot[0] % NW]
        wrot[0] += 1
        kc = rows // P
        cx.dma("pool", s.t[:, 0:kc, 0:ncols], src_buf.t[0:rows, c0:c0 + ncols].rearrange("(c p) f -> p c f", p=P),
               reads=[src_buf], writes=[s])
        return s

    class Bump:
        def __init__(self, start=0):
            self.o = start

        def take(self, shape, dt):
            esz = 4 if dt in (F32, I32) else 2
            n = esz
            for v in shape[1:]:
                n *= v
            o = self.o
            self.o += ((n + 2047) // 2048) * 2
            return A(o, shape, dt)

    b0 = Bump(0)
    posi = b0.take([P, NT2], I32)
    cx.dma("sp", posi[:], pos.t[0:1, :].partition_broadcast(P)[:, 0, :], reads=[pos], writes=[posi])
    posf = b0.take([P, NT2], F32)
    cx.op("dve", lambda e: e.tensor_copy(posf[:], posi[:]), reads=[posi], writes=[posf])
    tmpa = b0.take([P, NT2], F32)
    tmpb = b0.take([P, NT2], F32)
    tmpi = b0.take([P, NT2], I32)
    tabs = {}
    bK = Bump(c.KOFF_KB)
    TWO_PI = float(2 * np.pi)
    for nm, col, npart in (("64", 0, 64), ("128", 1, 128)):
        for kind, shift in (("cos", 0.5 * np.pi), ("sin", 0.0)):
            tb = bK.take([npart, NT2], BF16)
            cx.op("dve", lambda e: e.tensor_scalar(tmpa.t[0:npart, :], posf.t[0:npart, :], cf.t[0:npart, col:col + 1], float(shift),
                                                   op0=ALU.mult, op1=ALU.add), reads=[posf, cf], writes=[tmpa])
            cx.op("dve", lambda e: e.tensor_scalar(tmpb.t[0:npart, :], tmpa.t[0:npart, :], 1.0 / TWO_PI, None, op0=ALU.mult), reads=[tmpa], writes=[tmpb])
            cx.op("dve", lambda e: e.tensor_copy(tmpi.t[0:npart, :], tmpb.t[0:npart, :]), reads=[tmpb], writes=[tmpi])
            cx.op("dve", lambda e: e.tensor_copy(tmpb.t[0:npart, :], tmpi.t[0:npart, :]), reads=[tmpi], writes=[tmpb])
            cx.op("dve", lambda e: e.scalar_tensor_tensor(out=tmpa.t[0:npart, :], in0=tmpb.t[0:npart, :], scalar=-TWO_PI, in1=tmpa.t[0:npart, :],
                                                          op0=ALU.mult, op1=ALU.add), reads=[tmpb], writes=[tmpa])
            cx.op("dve", lambda e: e.tensor_scalar(tmpb.t[0:npart, :], tmpa.t[0:npart, :], float(np.pi), -TWO_PI, op0=ALU.is_gt, op1=ALU.mult),
                  reads=[tmpa], writes=[tmpb])
            cx.op("dve", lambda e: e.tensor_tensor(tmpa.t[0:npart, :], tmpa.t[0:npart, :], tmpb.t[0:npart, :], ALU.add), reads=[tmpb], writes=[tmpa])
            if kind == "cos":
                cx.op("act", lambda e: e.activation(out=tb[:], in_=tmpa.t[0:npart, :], func=AF.Sin), reads=[tmpa], writes=[tb])
            else:
                cx.op("act", lambda e: e.activation(out=tmpa.t[0:npart, :], in_=tmpa.t[0:npart, :], func=AF.Sin), reads=[], writes=[tmpa])
                cx.op("dve", lambda e: e.tensor_scalar(tb[:], tmpa.t[0:npart, :], cf.t[0:npart, 2 + col:3 + col], None, op0=ALU.mult),
                      reads=[tmpa, cf], writes=[tb])
            tabs[kind + nm] = tb

    snk = sb("snk", [P, HS], F32)
    cx.dma("sp", snk[:], sink.t[0:1, :].partition_broadcast(P)[:, 0, :], reads=[sink], writes=[snk])
    esnk = sb("esnk", [P, HS], F32)
    cx.op("act", lambda e: e.activation(out=esnk[:], in_=snk[:], func=AF.Exp), reads=[snk], writes=[esnk])

    qn_g = sb("qn_g", [P, QRC], F32); kn_g = sb("kn_g", [P, KRC], F32)
    cx.dma("sp", qn_g[:], q_norm[:], reads=[q_norm], writes=[qn_g])
    cx.dma("sp", kn_g[:], kv_norm[:], reads=[kv_norm], writes=[kn_g])

    if c.STOP == "setup":
        cx.finish()
        return nc
    ckvnT = bK.take([P, KRC, NT2], BF16)
    krT = bK.take([64, NT2], BF16)
    kbT = bK.take([P, HKV, (NB + 2) * P], BF16)
    vb = bK.take([P, NB + 2, HKV, 129], BF16)
    cx.op("pool", lambda e: e.memset(vb[:], 1.0), writes=[vb])

    bP = Bump(0)
    xT = bP.take([P, DK, CH], BF16)
    oaT = bP.take([P, HVC, CH], BF16)
    obT = bP.take([P, HSC, CH], BF16)
    y1 = A(0, [P, NQT, D], F32)
    bP.o = max(bP.o, ((NQT * D * 4 + 2047) // 2048) * 2)
    o_xbt = bP.o
    raw = bP.take([P, max(QRC, KRC), CH], F32)
    cqnT = bP.take([P, QRC, CH], BF16)
    qbT = bP.take([P, NQT, HS, P], BF16)
    xbt = A(o_xbt, [P, NQT, D], BF16)
    bP.o = max(bP.o, o_xbt + ((NQT * D * 2 + 2047) // 2048) * 2)
    mT = bP.take([P, DK, CH], BF16)
    knh = [bP.take([P, NT2], BF16)]
    vah = [bP.take([P, NT2 // P, 129], BF16)]
    wq_h = [bP.take([P, QRC, QW], BF16)]
    wkv_h = [bP.take([P, KRC, KW], BF16)]
    assert bP.o <= c.KOFF_KB, (bP.o, c.KOFF_KB)
    sqb = sb("sqb", [P, CH], BF16)
    rstd = sb("rstd", [P, CH], F32)
    r16 = sb("r16", [P, CH], BF16)
    rt1 = sb("rt1", [P, CH], F32)
    rt2 = sb("rt2", [P, CH], F32)

    def make_xT(src_buf, t0):
        cx.dma("pool", xbt[:], src_buf.t[t0:t0 + CH, :].rearrange("(j p) d -> p j d", p=P), reads=[src_buf], writes=[xbt])
        transpose_to(xbt, xT)

    def transpose_to(srcb, dstT):
        for j in range(NQT):
            for k0 in range(0, DK, 8):
                kn = min(8, DK - k0)
                for k in range(kn):
                    cx.transpose(PT.t[:, k * P:(k + 1) * P], srcb.t[:, j, (k0 + k) * P:(k0 + k + 1) * P], ident,
                                 reads=[srcb, cbf], pbuf=PT, pw=(k > 0))
                cx.op("act", lambda e: e.copy(out=dstT.t[:, k0:k0 + kn, j * P:(j + 1) * P],
                                              in_=PT.t[:, 0:kn * P].rearrange("p (k t) -> p k t", t=P)),
                      reads=[PT], pwrites=[dstT])

    def proj_fm(wsrc, rows, c0, ncols, actT, kc, ntok, pbank):
        s = wload(wsrc, rows, c0, ncols)
        cx.mm(pbank.t[0:ncols, 0:ntok], [(s.t[:, k, 0:ncols], actT.t[:, k, 0:ntok]) for k in range(kc)],
              reads=[s, actT], pbuf=pbank)
        return pbank

    def rms_finish(nchunks, feat, gcol, dstT, tcol0, ntok):
        for ch in range(nchunks):
            cx.op("act", lambda e: e.activation(out=sqb.t[:, 0:ntok], in_=raw.t[:, ch, 0:ntok], func=AF.Square), reads=[raw], writes=[sqb])
            cx.mm(PM.t[:, 0:ntok], [(ones_bf, sqb.t[:, 0:ntok])], reads=[sqb, cbf], pbuf=PM, pw=(ch > 0)) if False else None
        return None

    def rms_norm_T(nchunks, feat, gcol, dstT, tcol0, ntok):
        for ch in range(nchunks):
            cx.op("act", lambda e: e.activation(out=sqb.t[:, 0:ntok], in_=raw.t[:, ch, 0:ntok], func=AF.Square), reads=[raw], writes=[sqb])
            cx.mm(PM.t[:, 0:ntok], [(ones_bf, sqb.t[:, 0:ntok])], reads=[sqb, cbf], pbuf=PM)
            if ch == 0:
                cx.op("dve", lambda e: e.tensor_copy(rstd.t[:, 0:ntok], PM.t[:, 0:ntok]), reads=[PM], writes=[rstd])
            else:
                cx.op("dve", lambda e: e.tensor_tensor(rstd.t[:, 0:ntok], rstd.t[:, 0:ntok], PM.t[:, 0:ntok], ALU.add), reads=[PM], writes=[rstd])
        cx.op("dve", lambda e: e.tensor_scalar(rstd.t[:, 0:ntok], rstd.t[:, 0:ntok], 1.0 / feat, float(c.rms_eps), op0=ALU.mult, op1=ALU.add),
              reads=[], writes=[rstd])
        cx.op("act", lambda e: e.activation(out=rstd.t[:, 0:ntok], in_=rstd.t[:, 0:ntok], func=AF.Sqrt), reads=[], writes=[rstd])
        cx.op("dve", lambda e: e.reciprocal(rstd.t[:, 0:ntok], rstd.t[:, 0:ntok]), reads=[], writes=[rstd])
        for ch in range(nchunks):
            cx.op("dve", lambda e: e.scalar_tensor_tensor(out=dstT.t[:, ch, tcol0:tcol0 + ntok], in0=raw.t[:, ch, 0:ntok],
                                                          scalar=gcol.t[:, ch:ch + 1], in1=rstd.t[:, 0:ntok], op0=ALU.mult, op1=ALU.mult),
                  reads=[raw, rstd, gcol], pwrites=[dstT])

    ra16 = sb("ra16", [P, CH], BF16)

    def rope_T(pbank, npart, ntok, tabn, tcol0, dst_ap, dstbuf):
        cosb, sinb = tabs["cos" + tabn], tabs["sin" + tabn]
        sw = swap64 if npart == 64 else swap128
        cx.op("dve", lambda e: e.tensor_tensor(ra16.t[0:npart, 0:ntok], pbank.t[0:npart, 0:ntok], cosb.t[:, tcol0:tcol0 + ntok], ALU.mult),
              reads=[pbank, cosb], writes=[ra16])
        cx.op("dve", lambda e: e.tensor_tensor(r16.t[0:npart, 0:ntok], pbank.t[0:npart, 0:ntok], sinb.t[:, tcol0:tcol0 + ntok], ALU.mult),
              reads=[pbank, sinb], writes=[r16])
        cx.mm(PM.t[0:npart, 0:ntok], [(cbf.t[0:npart, 0:npart], ra16.t[0:npart, 0:ntok]), (sw, r16.t[0:npart, 0:ntok])],
              reads=[ra16, r16, cbf], pbuf=PM)
        cx.op("act", lambda e: e.copy(out=dst_ap, in_=PM.t[0:npart, 0:ntok]), reads=[PM], pwrites=[dstbuf])

    for ci in range(2 * NPASS):
        own = ci < NPASS
        src = x_own if own else x_oth
        t0 = (ci % NPASS) * CH
        g0 = ci * CH
        make_xT(src, t0)
        if c.STOP == "kv1":
            cx.finish()
            return nc
        for ch in range(KRC):
            pb = proj_fm(w_in, D, o_ckv + ch * P, P, xT, DK, CH, bankAB())
            cx.op("act", lambda e: e.copy(out=raw.t[:, ch, :], in_=pb.t[:, 0:CH]), reads=[pb], pwrites=[raw] if ch else (), writes=() if ch else [raw])
        if c.STOP == "kv2":
            cx.finish()
            return nc
        rms_norm_T(KRC, c.KR, kn_g, ckvnT, g0, CH)
        if c.STOP == "kv3":
            cx.finish()
            return nc
        pb = proj_fm(w_in, D, o_kr, 64, xT, DK, CH, bankAB())
        rope_T(pb, 64, CH, "64", g0, krT.t[:, g0:g0 + CH], krT)
        if c.STOP == "kv4":
            cx.finish()
            return nc
        for j in range(NQT):
            blk = (ci % NPASS) * NQT + j
            if own:
                slot = blk
            elif blk == NB - 1:
                slot = NB
            elif blk == 0:
                slot = NB + 1
            else:
                continue
            for kv in range(HKV):
                s = wload(w_in, D, o_kb + kv * P, P)
                pbk = bankAB()
                cx.mm(pbk.t[:, 0:P], [(s.t[:, k, :], xT.t[:, k, j * P:(j + 1) * P]) for k in range(DK)], reads=[s, xT], pbuf=pbk)
                if c.STOP == "kv5a":
                    cx.finish()
                    return nc
                rope_T(pbk, 128, P, "128", g0 + j * P, kbT.t[:, kv, slot * P:(slot + 1) * P], kbT)
                if c.STOP == "kv5b" or (c.STOP and c.STOP.startswith("r")):
                    cx.finish()
                    return nc
                s = wload(w_in, D, o_vb + kv * P, P)
                pbk = bankAB()
                cx.mm(pbk.t[:, 0:P], [(xT.t[:, k, j * P:(j + 1) * P], s.t[:, k, :]) for k in range(DK)], reads=[s, xT], pbuf=pbk)
                cx.op("act", lambda e: e.copy(out=vb.t[:, slot, kv, 0:128], in_=pbk.t[:, 0:P]), reads=[pbk], pwrites=[vb])
                if c.STOP == "kv5":
                    cx.finish()
                    return nc
        if c.STOP == "kv6":
            cx.finish()
            return nc

    if c.STOP == "kv":
        cx.finish()
        return nc
    qn = [sb(f"qn{i}", [P, CH], BF16) for i in range(2)]
    qr = [sb(f"qr{i}", [64, CH], BF16) for i in range(2)]
    for v in vah:
        cx.op("pool", lambda e: e.memset(v[:], 1.0), writes=[v])
    Et = [sb(f"E{i}", [P, 512], BF16) for i in range(3)]
    erot = [0]
    rc = sb("rc", [P, 8], F32)
    on = sb("on", [P, 4, P], BF16)
    sga = sb("sga", [P, CH], F32); t1 = sb("t1", [P, CH], F32); t2 = sb("t2", [P, CH], F32)
    lnw = sb("lnw", [P, 512], F32); lnb = sb("lnb", [P, 512], F32)
    stats = sb("stats", [P, max(1, D // 512), 6], F32); mv = sb("mv", [P, 2], F32); rs = sb("rs", [P, 1], F32)
    pbt = sb("pbt", [P, NQT, c.PLE], BF16); pT = sb("pT", [P, PK, CH], BF16)
    wrt = sb("wrt", [P, DK, NR], BF16)
    cx.dma("pool", wrt[:], w_rt.t.rearrange("(c p) f -> p c f", p=P), reads=[w_rt], writes=[wrt])
    brt = sb("brt", [P, NR], F32)
    cx.dma("sp", brt[:], b_rt.t[0:1, :].partition_broadcast(P)[:, 0, :], reads=[b_rt], writes=[brt])
    lg = sb("lg", [P, NB, NR], F32)
    plt = sb("plt", [P, 512], F32)

    def attn_finish(acc_ap, h_sink, dst_ap, dstbuf, slot):
        if h_sink is None:
            cx.op("dve", lambda e: e.reciprocal(rc.t[:, slot:slot + 1], acc_ap[:, 128:129]), reads=[PO], pwrites=[rc])
        else:
            cx.op("dve", lambda e: e.tensor_tensor(rc.t[:, slot:slot + 1], acc_ap[:, 128:129], esnk.t[:, h_sink:h_sink + 1], ALU.add),
                  reads=[PO, esnk], pwrites=[rc])
            cx.op("dve", lambda e: e.reciprocal(rc.t[:, slot:slot + 1], rc.t[:, slot:slot + 1]), reads=[], pwrites=[rc])
        cx.op("dve", lambda e: e.tensor_scalar(on.t[:, slot, :], acc_ap[:, 0:128], rc.t[:, slot:slot + 1], None, op0=ALU.mult),
              reads=[PO, rc], pwrites=[on])
        cx.transpose(PT.t[:, slot * P:(slot + 1) * P], on.t[:, slot, :], ident, reads=[on, cbf], pbuf=PT, pw=True)
        cx.op("act", lambda e: e.copy(out=dst_ap, in_=PT.t[:, slot * P:(slot + 1) * P]), reads=[PT], pwrites=[dstbuf])

    for p in range(NPASS):
        t0 = p * CH
        make_xT(x_own, t0)
        for ch in range(QRC):
            pb = proj_fm(w_in, D, o_cq + ch * P, P, xT, DK, CH, bankAB())
            cx.op("act", lambda e: e.copy(out=raw.t[:, ch, :], in_=pb.t[:, 0:CH]), reads=[pb], pwrites=[raw] if ch else (), writes=() if ch else [raw])
        rms_norm_T(QRC, c.QR, qn_g, cqnT, 0, CH)
        for hh in range(HS):
            s = wload(w_in, D, o_qb + hh * P, P)
            for j in range(NQT):
                pbk = bankAB()
                cx.mm(pbk.t[:, 0:P], [(s.t[:, k, :], xT.t[:, k, j * P:(j + 1) * P]) for k in range(DK)], reads=[s, xT], pbuf=pbk)
                rope_T(pbk, 128, P, "128", t0 + j * P, qbT.t[:, j, hh, :], qbT)
        for hh in range(H):
            b2 = 0
            cx.dma("pool", wq_h[b2][:], w_q_up.t[:, hh * QW:(hh + 1) * QW].rearrange("(c p) f -> p c f", p=P), reads=[w_q_up], writes=[wq_h[b2]])
            cx.dma("pool", wkv_h[b2][:], w_kv_up.t[:, hh * KW:(hh + 1) * KW].rearrange("(c p) f -> p c f", p=P), reads=[w_kv_up], writes=[wkv_h[b2]])
            pb = bankAB()
            cx.mm(pb.t[:, 0:CH], [(wq_h[b2].t[:, k, 0:128], cqnT.t[:, k, :]) for k in range(QRC)], reads=[wq_h[b2], cqnT], pbuf=pb)
            cx.op("act", lambda e: e.copy(out=qn[b2][:], in_=pb.t[:, 0:CH]), reads=[pb], writes=[qn[b2]])
            pb = bankAB()
            cx.mm(pb.t[0:64, 0:CH], [(wq_h[b2].t[:, k, 128:192], cqnT.t[:, k, :]) for k in range(QRC)], reads=[wq_h[b2], cqnT], pbuf=pb)
            rope_T(pb, 64, CH, "64", t0, qr[b2][:], qr[b2])
            for s0 in range(0, NT2, 512):
                sn = min(512, NT2 - s0)
                pb = bankAB()
                cx.mm(pb.t[:, 0:sn], [(wkv_h[b2].t[:, k, 0:128], ckvnT.t[:, k, s0:s0 + sn]) for k in range(KRC)], reads=[wkv_h[b2], ckvnT], pbuf=pb)
                cx.op("act", lambda e: e.copy(out=knh[b2].t[:, s0:s0 + sn], in_=pb.t[:, 0:sn]), reads=[pb], pwrites=[knh[b2]])
            for kt0 in range(0, NT2 // P, 4):
                pb = bankAB()
                for kk in range(4):
                    kt = kt0 + kk
                    cx.mm(pb.t[:, kk * P:(kk + 1) * P], [(ckvnT.t[:, k, kt * P:(kt + 1) * P], wkv_h[b2].t[:, k, 128:256]) for k in range(KRC)],
                          reads=[wkv_h[b2], ckvnT], pbuf=pb, pw=(kk > 0))
                cx.op("act", lambda e: e.copy(out=vah[b2].t[:, kt0:kt0 + 4, 0:128], in_=pb.t[:, 0:512].rearrange("p (k t) -> p k t", t=P)),
                      reads=[pb], pwrites=[vah[b2]])
            nkt = NT2 // P
            for kt in range(nkt):
                pS = bankS()
                cx.mm(pS.t[:, 0:CH], [(knh[b2].t[:, kt * P:(kt + 1) * P], qn[b2][:]), (krT.t[:, kt * P:(kt + 1) * P], qr[b2][:])],
                      reads=[knh[b2], qn[b2], krT, qr[b2]], pbuf=pS)
                Eb = Et[erot[0] % 3]; erot[0] += 1
                cx.op("act", lambda e: e.activation(out=Eb.t[:, 0:CH], in_=pS.t[:, 0:CH], func=AF.Exp, scale=sc_mla), reads=[pS], writes=[Eb])
                for j in range(NQT):
                    E_ = cx.engs["pe"]
                    cx._waits(E_, [Eb, vah[b2]], (PO,) if (kt == 0 and j == 0) else (), ())
                    ins = E_.e.matmul(PO.t[:, j * 512:j * 512 + 129], Eb.t[:, j * P:(j + 1) * P], vah[b2].t[:, kt, :], start=(kt == 0), stop=(kt == nkt - 1))
                    tok = E_.mark(ins)
                    cx._commit(tok, [Eb, vah[b2]], (), (PO,))
            for j in range(NQT):
                attn_finish(PO.t[:, j * 512:j * 512 + 129], None, oaT.t[:, hh, j * P:(j + 1) * P], oaT, j)
            cx.wait_all("pe", [])
        for j in range(NQT):
            jb = p * NQT + j
            blks = [((jb - 1) if jb > 0 else NB, 0 if jb > 0 else 2), (jb, None), ((jb + 1) if jb < NB - 1 else NB + 1, 1 if jb < NB - 1 else 3)]
            for kv in range(HKV):
              for gh in range(0, GRP, 2):
                ng = min(2, GRP - gh)
                for bi, (blk, mk) in enumerate(blks):
                    pS = bankS()
                    steps = [(kbT.t[:, kv, blk * P:(blk + 1) * P], qbT.t[:, j, kv * GRP + gh:kv * GRP + gh + ng, :].rearrange("p h t -> p (h t)"))]
                    if mk is not None:
                        steps.append((ident, masks[:, mk * 512:mk * 512 + ng * P]))
                    cx.mm(pS.t[:, 0:ng * P], steps, reads=[kbT, qbT, cbf], pbuf=pS)
                    Eb = Et[erot[0] % 3]; erot[0] += 1
                    cx.op("act", lambda e: e.activation(out=Eb.t[:, 0:ng * P], in_=pS.t[:, 0:ng * P], func=AF.Exp, scale=sc_swa), reads=[pS], writes=[Eb])
                    for gq in range(ng):
                        E_ = cx.engs["pe"]
                        cx._waits(E_, [Eb, vb], (PO,) if (bi == 0 and gq == 0) else (), ())
                        ins = E_.e.matmul(PO.t[:, gq * 512:gq * 512 + 129], Eb.t[:, gq * P:(gq + 1) * P], vb.t[:, blk, kv, :], start=(bi == 0), stop=(bi == 2))
                        tok = E_.mark(ins)
                        cx._commit(tok, [Eb, vb], (), (PO,))
                for gq in range(ng):
                    hq = kv * GRP + gh + gq
                    attn_finish(PO.t[:, gq * 512:gq * 512 + 129], hq, obT.t[:, hq, j * P:(j + 1) * P], obT, gq)
        for cc in range(DK):
            pb = proj_fm(w_in, D, o_ga + cc * P, P, xT, DK, CH, bankAB())
            cx.op("act", lambda e: e.activation(out=sga[:], in_=pb.t[:, 0:CH], func=AF.Sigmoid), reads=[pb], writes=[sga])
            pb2 = proj_fm(w_a, H * c.VD, cc * P, P, oaT, HVC, CH, bankAB())
            cx.op("dve", lambda e: e.tensor_tensor(t1[:], sga[:], pb2.t[:, 0:CH], ALU.mult), reads=[sga, pb2], writes=[t1])
            pb = proj_fm(w_in, D, o_gb + cc * P, P, xT, DK, CH, bankAB())
            cx.op("act", lambda e: e.activation(out=sga[:], in_=pb.t[:, 0:CH], func=AF.Sigmoid), reads=[pb], writes=[sga])
            pb2 = proj_fm(w_b, HS * c.HD, cc * P, P, obT, HSC, CH, bankAB())
            cx.op("dve", lambda e: e.tensor_tensor(t2[:], sga[:], pb2.t[:, 0:CH], ALU.mult), reads=[sga, pb2], writes=[t2])
            cx.op("dve", lambda e: e.tensor_tensor(mT.t[:, cc, :], t1[:], t2[:], ALU.add), reads=[t1, t2], pwrites=[mT])
        cx.dma("sp", y1[:], x_own.t[t0:t0 + CH, :].rearrange("(j p) d -> p j d", p=P), reads=[x_own], writes=[y1])
        for cb in range(DK):
            s = wload(w_out, D, cb * P, P)
            for j in range(NQT):
                pb = bankAB()
                cx.mm(pb.t[:, 0:P], [(mT.t[:, k, j * P:(j + 1) * P], s.t[:, k, :]) for k in range(DK)], reads=[s, mT], pbuf=pb)
                cx.op("dve", lambda e: e.scalar_tensor_tensor(out=y1.t[:, j, cb * P:(cb + 1) * P], in0=y1.t[:, j, cb * P:(cb + 1) * P],
                                                              scalar=float(c.alpha), in1=pb.t[:, 0:P], op0=ALU.mult, op1=ALU.add),
                      reads=[pb], writes=[y1])
        layer_norm(cx, c, y1, NQT, D, ln1_w, ln1_b, lnw, lnb, stats, mv, rs)
        cx.op("act", lambda e: e.copy(out=xbt[:], in_=y1[:]), reads=[y1], writes=[xbt])
        cx.dma("sp", x1_own_d.t[t0:t0 + CH, :].rearrange("(j p) d -> p j d", p=P), xbt[:], reads=[xbt], pwrites=[x1_own_d])
        transpose_to(xbt, mT)
        for j in range(NQT):
            pb = bankAB()
            cx.mm(pb.t[:, 0:NR], [(mT.t[:, k, j * P:(j + 1) * P], wrt.t[:, k, :]) for k in range(DK)], reads=[mT, wrt], pbuf=pb)
            cx.op("dve", lambda e: e.tensor_tensor(lg.t[:, p * NQT + j, :], pb.t[:, 0:NR], brt[:], ALU.add), reads=[pb, brt], pwrites=[lg])
        cx.dma("pool", pbt[:], p_own.t[t0:t0 + CH, :].rearrange("(j p) d -> p j d", p=P), reads=[p_own], writes=[pbt])
        for j in range(NQT):
            for k in range(PK):
                cx.transpose(PT.t[:, k * P:(k + 1) * P], pbt.t[:, j, k * P:(k + 1) * P], ident, reads=[pbt, cbf], pbuf=PT, pw=(k > 0))
            cx.op("act", lambda e: e.copy(out=pT.t[:, :, j * P:(j + 1) * P], in_=PT.t[:, 0:PK * P].rearrange("p (k t) -> p k t", t=P)),
                  reads=[PT], pwrites=[pT])
        for cb in range(DK):
            s = wload(w_pg, D, cb * P, P)
            s2 = wload(w_pu, c.PLE, cb * P, P)
            for j in range(NQT):
                pb = bankAB()
                cx.mm(pb.t[:, 0:P], [(mT.t[:, k, j * P:(j + 1) * P], s.t[:, k, :]) for k in range(DK)], reads=[s, mT], pbuf=pb)
                pb2 = bankAB()
                cx.mm(pb2.t[:, 0:P], [(pT.t[:, k, j * P:(j + 1) * P], s2.t[:, k, :]) for k in range(PK)], reads=[s2, pT], pbuf=pb2)
                cx.op("act", lambda e: e.activation(out=plt.t[:, 0:P], in_=pb.t[:, 0:P], func=AF.Sigmoid), reads=[pb], writes=[plt])
                cx.op("dve", lambda e: e.tensor_tensor(plt.t[:, 0:P], plt.t[:, 0:P], pb2.t[:, 0:P], ALU.mult), reads=[pb2], writes=[plt])
                cx.op("dve", lambda e: e.scalar_tensor_tensor(out=y1.t[:, j, cb * P:(cb + 1) * P], in0=y1.t[:, j, cb * P:(cb + 1) * P],
                                                              scalar=float(c.alpha), in1=plt.t[:, 0:P], op0=ALU.mult, op1=ALU.add),
                      reads=[plt], writes=[y1])
        cx.dma("sp", base_d.t[t0:t0 + CH, :].rearrange("(j p) d -> p j d", p=P), y1[:], reads=[y1], pwrites=[base_d])

    if c.STOP == "pass":
        cx.finish()
        return nc
    GE = c.G * c.E
    cmb = sb("cmb", [P, NB, GE], F32)
    rtmp = sb("rtmp", [P, 64], F32)
    gm = sb("gm", [P, 8], F32)
    goh = sb("goh", [P, c.G], F32); sel = sb("sel", [P, c.E], F32); oh1 = sb("oh1", [P, c.E], F32); oh2 = sb("oh2", [P, c.E], F32)
    wi = sb("wi", [P, c.E], F32); prod = sb("prod", [P, c.E, c.G], F32)
    G_, E_n = c.G, c.E
    for i in range(NB):
        L = lg.t[:, i, :]
        cx.op("dve", lambda e: e.tensor_reduce(out=gm.t[:, 0:1], in_=L[:, 0:G_], axis=AX.X, op=ALU.max), reads=[lg], writes=[gm])
        cx.op("dve", lambda e: e.tensor_scalar(goh[:], L[:, 0:G_], gm.t[:, 0:1], None, op0=ALU.is_equal), reads=[lg, gm], writes=[goh])
        cx.op("dve", lambda e: e.tensor_scalar(rtmp.t[:, 0:G_], L[:, 0:G_], gm.t[:, 0:1], None, op0=ALU.subtract), reads=[lg, gm], writes=[rtmp])
        cx.op("act", lambda e: e.activation(out=rtmp.t[:, 0:G_], in_=rtmp.t[:, 0:G_], func=AF.Exp), reads=[], writes=[rtmp])
        cx.op("dve", lambda e: e.tensor_reduce(out=gm.t[:, 1:2], in_=rtmp.t[:, 0:G_], axis=AX.X, op=ALU.add), reads=[rtmp], writes=[gm])
        cx.op("dve", lambda e: e.reciprocal(gm.t[:, 1:2], gm.t[:, 1:2]), reads=[], writes=[gm])
        cx.op("dve", lambda e: e.tensor_tensor(prod[:], L[:, G_:G_ + GE].rearrange("p (g e) -> p e g", g=G_),
                                               goh[:].unsqueeze(1).to_broadcast([P, E_n, G_]), ALU.mult), reads=[lg, goh], writes=[prod])
        cx.op("dve", lambda e: e.tensor_reduce(out=sel[:], in_=prod[:], axis=AX.X, op=ALU.add), reads=[prod], writes=[sel])
        cx.op("dve", lambda e: e.tensor_reduce(out=gm.t[:, 2:3], in_=sel[:], axis=AX.X, op=ALU.max), reads=[sel], writes=[gm])
        cx.op("dve", lambda e: e.tensor_scalar(oh1[:], sel[:], gm.t[:, 2:3], None, op0=ALU.is_equal), reads=[sel, gm], writes=[oh1])
        cx.op("dve", lambda e: e.scalar_tensor_tensor(out=rtmp.t[:, 8:8 + E_n], in0=oh1[:], scalar=-1.0e30, in1=sel[:], op0=ALU.mult, op1=ALU.add),
              reads=[oh1, sel], writes=[rtmp])
        cx.op("dve", lambda e: e.tensor_reduce(out=gm.t[:, 3:4], in_=rtmp.t[:, 8:8 + E_n], axis=AX.X, op=ALU.max), reads=[rtmp], writes=[gm])
        cx.op("dve", lambda e: e.tensor_scalar(oh2[:], rtmp.t[:, 8:8 + E_n], gm.t[:, 3:4], None, op0=ALU.is_equal), reads=[rtmp, gm], writes=[oh2])
        cx.op("dve", lambda e: e.tensor_tensor(gm.t[:, 4:5], gm.t[:, 3:4], gm.t[:, 2:3], ALU.subtract), reads=[], writes=[gm])
        cx.op("act", lambda e: e.activation(out=gm.t[:, 4:5], in_=gm.t[:, 4:5], func=AF.Exp), reads=[gm], writes=[gm])
        cx.op("dve", lambda e: e.tensor_scalar(gm.t[:, 5:6], gm.t[:, 4:5], 1.0, None, op0=ALU.add), reads=[gm], writes=[gm])
        cx.op("dve", lambda e: e.reciprocal(gm.t[:, 5:6], gm.t[:, 5:6]), reads=[], writes=[gm])
        cx.op("dve", lambda e: e.tensor_tensor(gm.t[:, 5:6], gm.t[:, 5:6], gm.t[:, 1:2], ALU.mult), reads=[], writes=[gm])
        cx.op("dve", lambda e: e.tensor_tensor(gm.t[:, 6:7], gm.t[:, 5:6], gm.t[:, 4:5], ALU.mult), reads=[], writes=[gm])
        cx.op("dve", lambda e: e.tensor_scalar(wi[:], oh1[:], gm.t[:, 5:6], None, op0=ALU.mult), reads=[oh1, gm], writes=[wi])
        cx.op("dve", lambda e: e.scalar_tensor_tensor(out=wi[:], in0=oh2[:], scalar=gm.t[:, 6:7], in1=wi[:], op0=ALU.mult, op1=ALU.add),
              reads=[oh2, gm], writes=[wi])
        cx.op("dve", lambda e: e.tensor_tensor(cmb.t[:, i, :].rearrange("p (g e) -> p g e", g=G_),
                                               goh[:].unsqueeze(2).to_broadcast([P, G_, E_n]),
                                               wi[:].unsqueeze(1).to_broadcast([P, G_, E_n]), ALU.mult), reads=[goh, wi], pwrites=[cmb])
    cx.dma("sp", cmb_own_d.t.rearrange("(j p) q -> p j q", p=P), cmb[:], reads=[cmb], writes=[cmb_own_d])

    if c.STOP == "route":
        cx.finish()
        return nc
    def collective(kind, op_, src, dst):
        E = cx.engs["pool"]
        cx._waits(E, [src], [dst], ())
        assert "coll" not in dst.dsem
        dst.dsem["coll"] = nc.alloc_semaphore(f"d{cx.nsem}"); cx.nsem += 1
        dst.dcnt["coll"] = 1
        cx.dma_bufs.append((dst, "coll", 1))
        ins = E.e.collective_compute(kind, op_, replica_groups=[list(range(NCORES))], ins=[src.t], outs=[dst.t])
        ins.then_inc(dst.dsem["coll"], 1)
        tok = (dst.dsem["coll"], 1)
        cx._commit(tok, [src], [dst], ())

    collective("AllGather", ALU.bypass, x1_own_d, x1_all_d)
    collective("AllGather", ALU.bypass, cmb_own_d, cmb_all_d)

    if c.STOP == "ag":
        cx.finish()
        return nc
    def moe_dense():
        NTT = TB // P
        bM = Bump(0)
        xsT = bM.take([P, DK, TB], BF16)
        hT = bM.take([P, c.E * FC, TB], BF16)
        stg = [bM.take([P, D], BF16) for _ in range(2)]
        wd = [bM.take([P, FC, 512], BF16) for _ in range(2)]
        acc = bM.take([P, NTT, 512], F32)
        cm64 = bM.take([P, NTT, GE], F32)
        cmy = sb("cmy", [P, NTT, c.E], F32)
        sg = bM.take([P, TB], F32)
        gselv = sb("gselv", [P, c.G], F32)
        cx.dma("sp", gselv[:], g_sel.t[0:1, :].partition_broadcast(P)[:, 0, :], reads=[g_sel], writes=[gselv])
        NCB = max(1, D // 512)
        CBW = min(512, D)
        for tb_ in range(NTB):
            tg0 = tb_ * TB
            for j in range(NTT):
                st = stg[j % 2]
                cx.dma("sp", st[:], x1_all_d.t[tg0 + j * P:tg0 + (j + 1) * P, :], reads=[x1_all_d], writes=[st])
                for k0 in range(0, DK, 8):
                    kn = min(8, DK - k0)
                    for k in range(kn):
                        cx.transpose(PT.t[:, k * P:(k + 1) * P], st.t[:, (k0 + k) * P:(k0 + k + 1) * P], ident, reads=[st, cbf], pbuf=PT, pw=(k > 0))
                    cx.op("act", lambda e: e.copy(out=xsT.t[:, k0:k0 + kn, j * P:(j + 1) * P], in_=PT.t[:, 0:kn * P].rearrange("p (k t) -> p k t", t=P)),
                          reads=[PT], pwrites=[xsT])
            cx.dma("sp", cm64[:], cmb_all_d.t[tg0:tg0 + TB, :].rearrange("(j p) q -> p j q", p=P), reads=[cmb_all_d], writes=[cm64])
            for j in range(NTT):
                cx.op("dve", lambda e: e.tensor_tensor(prod[:], cm64.t[:, j, :].rearrange("p (g e) -> p e g", g=G_),
                                                       gselv[:].unsqueeze(1).to_broadcast([P, E_n, G_]), ALU.mult), reads=[cm64, gselv], writes=[prod])
                cx.op("dve", lambda e: e.tensor_reduce(out=cmy.t[:, j, :], in_=prod[:], axis=AX.X, op=ALU.add), reads=[prod], pwrites=[cmy])
            for ex in range(c.E):
                for fc in range(FC):
                    s = wload_e(cx, wslots, wrot, w_gate, ex, D, fc * P, P)
                    pg = bankAB()
                    cx.mm(pg.t[:, 0:TB], [(s.t[:, k, :], xsT.t[:, k, :]) for k in range(DK)], reads=[s, xsT], pbuf=pg)
                    s2 = wload_e(cx, wslots, wrot, w_up, ex, D, fc * P, P)
                    pu = bankS()
                    cx.mm(pu.t[:, 0:TB], [(s2.t[:, k, :], xsT.t[:, k, :]) for k in range(DK)], reads=[s2, xsT], pbuf=pu)
                    cx.op("act", lambda e: e.activation(out=sg[:], in_=pg.t[:, 0:TB], func=AF.Silu), reads=[pg], writes=[sg])
                    cx.op("dve", lambda e: e.tensor_tensor(hT.t[:, ex * FC + fc, :], sg[:], pu.t[:, 0:TB], ALU.mult), reads=[sg, pu], pwrites=[hT])
            for cb in range(NCB):
                for ex in range(c.E):
                    wdb = wd[(cb * c.E + ex) % 2]
                    cx.dma("pool", wdb.t[:, :, 0:CBW], w_down.t[ex, :, cb * CBW:(cb + 1) * CBW].rearrange("(c p) f -> p c f", p=P), reads=[w_down], writes=[wdb])
                    for j in range(NTT):
                        pd = bankAB() if (j % 2 == 0) else bankS()
                        cx.mm(pd.t[:, 0:CBW], [(hT.t[:, ex * FC + fc, j * P:(j + 1) * P], wdb.t[:, fc, 0:CBW]) for fc in range(FC)], reads=[hT, wdb], pbuf=pd)
                        if ex == 0:
                            cx.op("dve", lambda e: e.tensor_scalar(acc.t[:, j, 0:CBW], pd.t[:, 0:CBW], cmy.t[:, j, ex:ex + 1], None, op0=ALU.mult),
                                  reads=[pd, cmy], pwrites=[acc])
                        else:
                            cx.op("dve", lambda e: e.scalar_tensor_tensor(out=acc.t[:, j, 0:CBW], in0=pd.t[:, 0:CBW], scalar=cmy.t[:, j, ex:ex + 1],
                                                                          in1=acc.t[:, j, 0:CBW], op0=ALU.mult, op1=ALU.add),
                                  reads=[pd, cmy], pwrites=[acc])
                cx.dma("sp", z_d.t[tg0:tg0 + TB, cb * CBW:(cb + 1) * CBW].rearrange("(j p) q -> p j q", p=P), acc.t[:, :, 0:CBW], reads=[acc], pwrites=[z_d])


    def moe_sparse():
        CAP = c.CAP
        NST = CAP // P
        NTI = NTOT // P
        NSLOT = c.E * CAP
        BIG = 1.0e6
        E8 = c.E
        bnd = nc.gpsimd.alloc_register("bnd")
        nc.gpsimd.reg_mov(bnd, NSLOT - 1)
        xs_d = dscr("xs_d", [NSLOT + P, D], BF16)
        ys_d = dscr("ys_d", [NSLOT + P, D], F32)
        dummy = sb("dmyrow", [P, 1], F32)
        cx.op("dve", lambda e: e.tensor_scalar(dummy[:], cf.t[:, 4:5], float(NSLOT), None, op0=ALU.add), reads=[cf], writes=[dummy])
        gselv = sb("gselv", [P, c.G], F32)
        cx.dma("sp", gselv[:], g_sel.t[0:1, :].partition_broadcast(P)[:, 0, :], reads=[g_sel], writes=[gselv])
        ecap = sb("ecap", [P, E8], F32)
        cx.dma("sp", ecap[:], e_cap.t[0:1, :].partition_broadcast(P)[:, 0, :], reads=[e_cap], writes=[ecap])
        bM = Bump(0)
        cmA = bM.take([P, NTI, GE], F32)
        cmyA = bM.take([P, NTI, E8], F32)
        Mf = bM.take([P, NTI, E8], F32)
        Mb = sb("Mb", [P, NTI * E8], BF16)
        rk = bM.take([P, NTI, E8], F32)
        tot = bM.take([P, NTI, E8], F32)
        exc = bM.take([P, NTI, E8], F32)
        cp = bM.take([P, NTI, E8], F32)
        val = bM.take([P, NTI, E8], F32)
        slv = bM.take([P, NTI, E8], F32)
        tA = bM.take([P, NTI, E8], F32)
        posf_ = {k: sb("posf_" + k, [P, NTI], F32) for k in ("lo", "hi")}
        posi_ = {k: sb("posi_" + k, [P, NTI], I32) for k in ("lo", "hi")}
        wsel = {k: sb("wsel_" + k, [P, NTI], F32) for k in ("lo", "hi")}
        o_exp = bM.o
        cx.dma("sp", cmA[:], cmb_all_d.t.rearrange("(j p) q -> p j q", p=P), reads=[cmb_all_d], writes=[cmA])
        for i in range(NTI):
            cx.op("dve", lambda e: e.tensor_tensor(prod[:], cmA.t[:, i, :].rearrange("p (g e) -> p e g", g=G_),
                                                   gselv[:].unsqueeze(1).to_broadcast([P, E_n, G_]), ALU.mult), reads=[cmA, gselv], writes=[prod])
            cx.op("dve", lambda e: e.tensor_reduce(out=cmyA.t[:, i, :], in_=prod[:], axis=AX.X, op=ALU.add), reads=[prod], pwrites=[cmyA])
        if c.STOP == "sp0a":
            return True
        cx.op("dve", lambda e: e.tensor_scalar(Mf[:], cmyA[:], 0.0, None, op0=ALU.is_gt), reads=[cmyA], writes=[Mf])
        if c.STOP == "sp0a1":
            return True
        cx.op("dve", lambda e: e.tensor_copy(Mb[:], Mf[:].rearrange("p a b -> p (a b)")), reads=[Mf], writes=[Mb])
        NC_ = NTI * E8
        if c.STOP == "sp0a2":
            return True
        cx.mm(PA.t[:, 0:NC_], [(utri, Mb[:])], reads=[Mb, cbf], pbuf=PA)
        if c.STOP == "sp0a3":
            return True
        cx.mm(PB.t[:, 0:NC_], [(ones_bf, Mb[:])], reads=[Mb, cbf], pbuf=PB)
        cx.op("act", lambda e: e.copy(out=tot[:].rearrange("p a b -> p (a b)"), in_=PB.t[:, 0:NC_]), reads=[PB], writes=[tot])
        if c.STOP == "sp0b":
            return True
        cx.op("dve", lambda e: e.memset(exc.t[:, 0, :], 0.0), writes=[exc])
        for i in range(1, NTI):
            cx.op("dve", lambda e: e.tensor_tensor(exc.t[:, i, :], exc.t[:, i - 1, :], tot.t[:, i - 1, :], ALU.add), reads=[tot], writes=[exc])
        cx.op("dve", lambda e: e.tensor_tensor(rk[:].rearrange("p a b -> p (a b)"), PA.t[:, 0:NC_], exc[:].rearrange("p a b -> p (a b)"), ALU.add),
              reads=[PA, exc], writes=[rk])
        if c.STOP == "sp0c":
            return True
        cx.op("dve", lambda e: e.memset(cp.t[:, :, 0:1], 0.0), writes=[cp])
        for ee in range(1, E8):
            cx.op("dve", lambda e: e.tensor_tensor(cp.t[:, :, ee:ee + 1], cp.t[:, :, ee - 1:ee], Mf.t[:, :, ee - 1:ee], ALU.add), reads=[Mf], writes=[cp])
        if c.STOP == "sp0d":
            return True
        cx.op("dve", lambda e: e.tensor_scalar(val[:], rk[:], float(CAP) - 0.5, None, op0=ALU.is_lt), reads=[rk], writes=[val])
        cx.op("dve", lambda e: e.tensor_tensor(val[:], val[:], Mf[:], ALU.mult), reads=[Mf], writes=[val])
        cx.op("dve", lambda e: e.tensor_tensor(slv[:], rk[:], ecap[:].unsqueeze(1).to_broadcast([P, NTI, E8]), ALU.add), reads=[rk, ecap], writes=[slv])
        cx.op("dve", lambda e: e.tensor_scalar(slv[:], slv[:], dummy.t[:, 0:1], None, op0=ALU.subtract), reads=[dummy], writes=[slv])
        for k, thr_op in (("lo", ALU.is_lt), ("hi", ALU.is_gt)):
            cx.op("dve", lambda e: e.tensor_scalar(tA[:], cp[:], 0.5, None, op0=thr_op), reads=[cp], writes=[tA])
            cx.op("dve", lambda e: e.tensor_tensor(tA[:], tA[:], val[:], ALU.mult), reads=[val], writes=[tA])
            cx.op("dve", lambda e: e.tensor_tensor(tot[:], tA[:], slv[:], ALU.mult), reads=[tA, slv], writes=[tot])
            cx.op("dve", lambda e: e.tensor_reduce(out=posf_[k][:], in_=tot[:], axis=AX.X, op=ALU.add), reads=[tot], writes=[posf_[k]])
            cx.op("dve", lambda e: e.tensor_scalar(posf_[k][:], posf_[k][:], dummy.t[:, 0:1], None, op0=ALU.add), reads=[dummy], writes=[posf_[k]])
            cx.op("dve", lambda e: e.tensor_copy(posi_[k][:], posf_[k][:]), reads=[posf_[k]], writes=[posi_[k]])
            cx.op("dve", lambda e: e.tensor_tensor(tot[:], tA[:], cmyA[:], ALU.mult), reads=[tA, cmyA], writes=[tot])
            cx.op("dve", lambda e: e.tensor_reduce(out=wsel[k][:], in_=tot[:], axis=AX.X, op=ALU.add), reads=[tot], writes=[wsel[k]])
        if c.STOP == "sp1":
            return True
        bX = Bump(o_exp)
        stg = [bX.take([P, D], BF16) for _ in range(2)]
        xsT = bX.take([P, DK, CAP], BF16)
        hT = bX.take([P, FC, CAP], BF16)
        wd = [bX.take([P, FC, 512], BF16) for _ in range(2)]
        ypc = [bX.take([P, 512], F32) for _ in range(4)]
        sg = bX.take([P, CAP], F32)
        cx.op("dve", lambda e: e.memset(stg[0][:], 0.0), writes=[stg[0]])
        for r in range(NSLOT // P + 1):
            cx.dma("sp", xs_d.t[r * P:(r + 1) * P, :], stg[0][:], reads=[stg[0]], pwrites=[xs_d])
        if c.STOP == "sp1b":
            return True
        for i in range(NTI):
            st = stg[(i + 1) % 2]
            cx.dma("sp", st[:], x1_all_d.t[i * P:(i + 1) * P, :], reads=[x1_all_d], writes=[st])
            for k in ("lo", "hi"):
                cx.dma("pool", None, None, reads=[st, posi_[k]], pwrites=[xs_d], owner=xs_d,
                       fn=lambda e: e.indirect_dma_start(out=xs_d.t[:, :], out_offset=bass.IndirectOffsetOnAxis(ap=posi_[k].t[:, i:i + 1], axis=0),
                                                         in_=st.t[:, :], in_offset=None))
        if c.STOP == "sp2":
            return True
        NCB = max(1, D // 512)
        CBW = min(512, D)
        yrot = [0]
        for ex in range(c.E):
            for s_ in range(NST):
                st = stg[s_ % 2]
                cx.dma("sp", st[:], xs_d.t[ex * CAP + s_ * P:ex * CAP + (s_ + 1) * P, :], reads=[xs_d], writes=[st])
                for k0 in range(0, DK, 8):
                    kn = min(8, DK - k0)
                    for k in range(kn):
                        cx.transpose(PT.t[:, k * P:(k + 1) * P], st.t[:, (k0 + k) * P:(k0 + k + 1) * P], ident, reads=[st, cbf], pbuf=PT, pw=(k > 0))
                    cx.op("act", lambda e: e.copy(out=xsT.t[:, k0:k0 + kn, s_ * P:(s_ + 1) * P], in_=PT.t[:, 0:kn * P].rearrange("p (k t) -> p k t", t=P)),
                          reads=[PT], pwrites=[xsT])
            for fc in range(FC):
                s = wload_e(cx, wslots, wrot, w_gate, ex, D, fc * P, P)
                pg = bankAB()
                cx.mm(pg.t[:, 0:CAP], [(s.t[:, k, :], xsT.t[:, k, :]) for k in range(DK)], reads=[s, xsT], pbuf=pg)
                s2 = wload_e(cx, wslots, wrot, w_up, ex, D, fc * P, P)
                pu = bankS()
                cx.mm(pu.t[:, 0:CAP], [(s2.t[:, k, :], xsT.t[:, k, :]) for k in range(DK)], reads=[s2, xsT], pbuf=pu)
                cx.op("act", lambda e: e.activation(out=sg[:], in_=pg.t[:, 0:CAP], func=AF.Silu), reads=[pg], writes=[sg])
                cx.op("dve", lambda e: e.tensor_tensor(hT.t[:, fc, :], sg[:], pu.t[:, 0:CAP], ALU.mult), reads=[sg, pu], pwrites=[hT])
            for cb in range(NCB):
                wdb = wd[(ex * NCB + cb) % 2]
                cx.dma("pool", wdb.t[:, :, 0:CBW], w_down.t[ex, :, cb * CBW:(cb + 1) * CBW].rearrange("(c p) f -> p c f", p=P), reads=[w_down], writes=[wdb])
                for s_ in range(NST):
                    pd = bankAB() if (s_ % 2 == 0) else bankS()
                    cx.mm(pd.t[:, 0:CBW], [(hT.t[:, fc, s_ * P:(s_ + 1) * P], wdb.t[:, fc, 0:CBW]) for fc in range(FC)], reads=[hT, wdb], pbuf=pd)
                    yb = ypc[yrot[0] % 4]; yrot[0] += 1
                    cx.op("act", lambda e: e.copy(out=yb.t[:, 0:CBW], in_=pd.t[:, 0:CBW]), reads=[pd], writes=[yb])
                    cx.dma("sp", ys_d.t[ex * CAP + s_ * P:ex * CAP + (s_ + 1) * P, cb * CBW:(cb + 1) * CBW], yb.t[:, 0:CBW], reads=[yb], pwrites=[ys_d])
        if c.STOP == "sp3":
            return True
        bG = Bump(o_exp)
        gl = [bG.take([P, D], F32) for _ in range(2)]
        gh = [bG.take([P, D], F32) for _ in range(2)]
        for b_ in gl + gh:
            cx.op("dve", lambda e: e.memset(b_[:], 0.0), writes=[b_])
        cx.dma("sp", ys_d.t[NSLOT:NSLOT + P, :], gl[0][:], reads=[gl[0]], pwrites=[ys_d])
        for i in range(NTI):
            a, b_ = gl[i % 2], gh[i % 2]
            for k, dst in (("lo", a), ("hi", b_)):
                cx.dma("pool", None, None, reads=[ys_d, posi_[k]], writes=[dst],
                       fn=lambda e: e.indirect_dma_start(out=dst.t[:, :], out_offset=None, in_=ys_d.t[:, :],
                                                         in_offset=bass.IndirectOffsetOnAxis(ap=posi_[k].t[:, i:i + 1], axis=0)))
            cx.op("dve", lambda e: e.tensor_scalar(a[:], a[:], wsel["lo"].t[:, i:i + 1], None, op0=ALU.mult), reads=[wsel["lo"]], writes=[a])
            cx.op("dve", lambda e: e.scalar_tensor_tensor(out=a[:], in0=b_[:], scalar=wsel["hi"].t[:, i:i + 1], in1=a[:], op0=ALU.mult, op1=ALU.add),
                  reads=[b_, wsel["hi"]], writes=[a])
            cx.dma("sp", z_d.t[i * P:(i + 1) * P, :], a[:], reads=[a], pwrites=[z_d])

    if c.MOE == "dense":
        moe_dense()
    else:
        if moe_sparse():
            cx.finish()
            return nc

    if c.STOP == "moe":
        cx.finish()
        return nc
    collective("ReduceScatter", ALU.add, z_d, moe_own_d)
    if c.STOP == "rs":
        cx.finish()
        return nc

    yt = A(0, [P, 1, D], F32)
    mo = A(((D * 4 + 2047) // 2048) * 2, [P, D], F32)
    for i in range(NB):
        cx.dma("sp", yt.t[:, 0, :], base_d.t[i * P:(i + 1) * P, :], reads=[base_d], writes=[yt])
        cx.dma("sp", mo[:], moe_own_d.t[i * P:(i + 1) * P, :], reads=[moe_own_d], writes=[mo])
        cx.op("dve", lambda e: e.tensor_tensor(yt.t[:, 0, :], yt.t[:, 0, :], mo[:], ALU.add), reads=[mo], writes=[yt])
        layer_norm(cx, c, yt, 1, D, ln2_w, ln2_b, lnw, lnb, stats, mv, rs)
        cx.dma("sp", out_d.t[i * P:(i + 1) * P, :], yt.t[:, 0, :], reads=[yt], pwrites=[out_d])
    cx.finish()
    return nc


def wload_e(cx, wslots, wrot, src_buf, ex, rows, c0, ncols):
    s = wslots[wrot[0] % len(wslots)]
    wrot[0] += 1
    kc = rows // 128
    cx.dma("pool", s.t[:, 0:kc, 0:ncols], src_buf.t[ex, 0:rows, c0:c0 + ncols].rearrange("(c p) f -> p c f", p=128),
           reads=[src_buf], writes=[s])
    return s


def layer_norm(cx, c, y, ntiles, D, w_d, b_d, lnw, lnb, stats, mv, rs):
    P = 128
    nch = max(1, D // 512)
    cw = min(512, D)
    for j in range(ntiles):
        for q in range(nch):
            cx.op("dve", lambda e: e.bn_stats(stats.t[:, q, :], y.t[:, j, q * cw:(q + 1) * cw]), reads=[y], pwrites=[stats] if q else (), writes=() if q else [stats])
        cx.op("dve", lambda e: e.bn_aggr(mv[:], stats.t[:, 0:nch, :].rearrange("p a b -> p (a b)")), reads=[stats], writes=[mv])
        cx.op("dve", lambda e: e.tensor_scalar(rs[:], mv.t[:, 1:2], float(c.ln_eps), None, op0=ALU.add), reads=[mv], writes=[rs])
        cx.op("act", lambda e: e.activation(out=rs[:], in_=rs[:], func=AF.Sqrt), reads=[], writes=[rs])
        cx.op("dve", lambda e: e.reciprocal(rs[:], rs[:]), reads=[], writes=[rs])
        cx.op("dve", lambda e: e.tensor_scalar(y.t[:, j, :], y.t[:, j, :], mv.t[:, 0:1], rs.t[:, 0:1], op0=ALU.subtract, op1=ALU.mult),
              reads=[mv, rs], writes=[y])
    for q in range(nch):
        cx.dma("sp", lnw.t[:, 0:cw], w_d.t[0:1, q * cw:(q + 1) * cw].partition_broadcast(P)[:, 0, :], reads=[w_d], writes=[lnw])
        cx.dma("sp", lnb.t[:, 0:cw], b_d.t[0:1, q * cw:(q + 1) * cw].partition_broadcast(P)[:, 0, :], reads=[b_d], writes=[lnb])
        for j in range(ntiles):
            cx.op("dve", lambda e: e.tensor_tensor(y.t[:, j, q * cw:(q + 1) * cw], y.t[:, j, q * cw:(q + 1) * cw], lnw.t[:, 0:cw], ALU.mult),
                  reads=[lnw], writes=[y])
            cx.op("dve", lambda e: e.tensor_tensor(y.t[:, j, q * cw:(q + 1) * cw], y.t[:, j, q * cw:(q + 1) * cw], lnb.t[:, 0:cw], ALU.add),
                  reads=[lnb], writes=[y])


def make_consts(cfg, core):
    P = 128
    bf = np.zeros((P, 128 + 128 + 64 + 128 + 4 * 512 + 128), np.float32)
    bf[:, 2496:2624] = np.triu(np.ones((128, 128), np.float32), 1)
    bf[:, 0:128] = np.eye(128)
    bf[:, 128:256] = 1.0
    sw64 = np.zeros((64, 64), np.float32)
    for p in range(64):
        sw64[(p + 32) % 64, p] = 1.0
    bf[0:64, 256:320] = sw64
    sw128 = np.zeros((128, 128), np.float32)
    for p in range(128):
        sw128[(p + 64) % 128, p] = 1.0
    bf[:, 320:448] = sw128
    NEG = -30000.0
    kj = np.arange(128)[:, None]
    qi = np.arange(128)[None, :]
    m_prev = np.where(kj >= qi, 0.0, NEG)
    m_next = np.where(kj <= qi, 0.0, NEG)
    m_none = np.full((128, 128), NEG)
    h = core % 2
    kinds = [m_prev, m_next, m_prev if h == 1 else m_none, m_next if h == 0 else m_none]
    for i, m in enumerate(kinds):
        bf[:, 448 + i * 512:448 + (i + 1) * 512] = np.tile(m, (1, 4))
    f = np.zeros((P, 8), np.float32)
    f[:, 0] = cfg.theta ** (-(2.0 * (np.arange(128) % 32)) / 64.0)
    f[:, 1] = cfg.theta ** (-(2.0 * (np.arange(128) % 64)) / 128.0)
    f[:, 2] = np.where(np.arange(128) % 64 < 32, 1.0, -1.0)
    f[:, 3] = np.where(np.arange(128) < 64, 1.0, -1.0)
    f[:, 4] = np.arange(128)
    return bf.astype(ml_dtypes.bfloat16), f


def make_in_maps(cfg, inputs):
    c = cfg
    x = np.asarray(inputs["x"]); p = np.asarray(inputs["p"])[0]; positions = np.asarray(inputs["positions"])
    g = lambda k: np.ascontiguousarray(np.asarray(inputs[k])[0])
    NT = c.NT
    w_rt = np.ascontiguousarray(np.concatenate([g("w_group"), g("w_expert_router")], axis=1))
    b_rt = np.concatenate([g("b_group"), g("b_expert")])[None, :]
    wg, wu, wd = g("w_gate"), g("w_up"), g("w_down")
    shared = dict(w_in=g("w_in"), q_norm=np.ascontiguousarray(g("q_norm").reshape(-1, 128).T), kv_norm=np.ascontiguousarray(g("kv_norm").reshape(-1, 128).T), w_q_up=g("w_q_up"), w_kv_up=g("w_kv_up"),
                  sink=g("sink")[None, :], w_a=g("w_branch_a"), w_b=g("w_branch_b"), w_out=g("w_out"),
                  ln1_w=g("ln1_w")[None, :], ln1_b=g("ln1_b")[None, :], ln2_w=g("ln2_w")[None, :], ln2_b=g("ln2_b")[None, :],
                  w_pu=g("w_ple_up"), w_pg=g("w_ple_gate"))
    in_maps = []
    for core in range(NCORES):
        b, h = core // 2, core % 2
        own = slice(h * NT, (h + 1) * NT); oth = slice((1 - h) * NT, (2 - h) * NT)
        cbf, cf = make_consts(c, core)
        gs = np.zeros((1, c.G), np.float32)
        gs[0, core] = 1.0
        m = dict(shared)
        m.update(x_own=np.ascontiguousarray(x[b, own]), x_oth=np.ascontiguousarray(x[b, oth]),
                 pos=np.ascontiguousarray(np.concatenate([positions[b, own], positions[b, oth]])[None, :].astype(np.int32)),
                 p_own=np.ascontiguousarray(p[b, own]), w_rt=w_rt, b_rt=b_rt,
                 w_gate=np.ascontiguousarray(wg[core]), w_up=np.ascontiguousarray(wu[core]), w_down=np.ascontiguousarray(wd[core]),
                 c_bf=cbf, c_f=cf, g_sel=gs, e_cap=(np.arange(c.E, dtype=np.float32) * c.CAP)[None, :])
        in_maps.append(m)
    return in_maps


def run(cfg, inputs):
    c = cfg
    NT = c.NT
    nc = build(c)
    in_maps = make_in_maps(c, inputs)
    res = run_bass_kernel_spmd(nc, in_maps, core_ids=list(range(NCORES)))
    out = np.zeros((c.B, c.S, c.D), np.float32)
    for core in range(NCORES):
        b, h = core // 2, core % 2
        out[b, h * NT:(h + 1) * NT] = res.results[core]["out"]
    return out


def kernel(**inputs):
    return run(Cfg(), inputs)
```
